# Optimizing a Trainium2 kernel written in Bass

```python
import jax, jax.numpy as jnp
from jax import lax
import numpy as np

D_MODEL = 4096
BATCH = 1
SEQ = 16384
DEPTH = 1

A_WIDTH = D_MODEL // 2
A_GROUPS = 4
A_GROUP_DIM = A_WIDTH // A_GROUPS
A_CHUNK = 128
B_HEADS = 4
B_KEY_WIDTH = D_MODEL // 4
B_VAL_WIDTH = D_MODEL // 2
B_DK = B_KEY_WIDTH // B_HEADS
B_DV = B_VAL_WIDTH // B_HEADS
B_GATE_RANK = 16
B_GATE_TAU = 16.0
B_CHUNK = 64
N_EXPERTS = 32
TOP_K = 4
D_EXPERT = 1536
SWIGLU_LIMIT = 7.0
SWIGLU_ALPHA = 1.702
MOE_BLOCK = 256
LN_EPS = 1e-5
DEEPNORM_ALPHA = (2 * DEPTH) ** 0.25
DEEPNORM_BETA = (8 * DEPTH) ** -0.25
SPLITS = (A_WIDTH, A_WIDTH, B_KEY_WIDTH, B_KEY_WIDTH, B_VAL_WIDTH, B_VAL_WIDTH, B_GATE_RANK, D_MODEL, D_MODEL)
D_IN = sum(SPLITS)

kernel_name = 'hybrid_gmlp_gla_moe_deepnorm'


def _layer_norm(x, g, b):
    xf = x.astype(jnp.float32)
    mu = jnp.mean(xf, axis=-1, keepdims=True)
    var = jnp.mean(jnp.square(xf - mu), axis=-1, keepdims=True)
    y = (xf - mu) * lax.rsqrt(var + LN_EPS)
    return (y * g.astype(jnp.float32) + b.astype(jnp.float32)).astype(x.dtype)


def _spatial_gating(u, v, ln_g, ln_b, ws, bs):
    bsz, seq, _ = v.shape
    n_chunks = seq // A_CHUNK
    v = _layer_norm(v.reshape(bsz, seq, A_GROUPS, A_GROUP_DIM), ln_g, ln_b)
    v = v.reshape(bsz, n_chunks, A_CHUNK, A_GROUPS, A_GROUP_DIM)
    causal = jnp.tril(jnp.ones((A_CHUNK, A_CHUNK), dtype=bool))
    ws_c = jnp.where(causal, ws, 0).astype(v.dtype)
    mixed = jnp.einsum('gts,bcsgd->bctgd', ws_c, v) + bs.T[:, :, None].astype(v.dtype)
    return u * mixed.reshape(bsz, seq, A_WIDTH)


def _gla(q, k, v, lr, w_lr, b_lr):
    f32 = jnp.float32
    bsz, seq, _ = q.shape
    n = seq // B_CHUNK
    gate_logits = jnp.einsum('btr,rk->btk', lr.astype(f32), w_lr.astype(f32)) + b_lr.astype(f32)
    log_a = jax.nn.log_sigmoid(gate_logits) / B_GATE_TAU
    shp_k = (bsz, n, B_CHUNK, B_HEADS, B_DK)
    shp_v = (bsz, n, B_CHUNK, B_HEADS, B_DV)
    log_a = log_a.reshape(shp_k)
    q = q.astype(f32).reshape(shp_k) * (B_DK ** -0.5)
    k = k.astype(f32).reshape(shp_k)
    v = v.astype(f32).reshape(shp_v)
    cum = jnp.cumsum(log_a, axis=2)
    cum_last = cum[:, :, -1:]
    q_t = q * jnp.exp(cum)
    k_t = k * jnp.exp(-cum)
    k_last = k * jnp.exp(cum_last - cum)
    causal = jnp.tril(jnp.ones((B_CHUNK, B_CHUNK), dtype=bool))
    scores = jnp.where(causal, jnp.einsum('bnihd,bnjhd->bnhij', q_t, k_t), 0.0)
    o_intra = jnp.einsum('bnhij,bnjhv->bnihv', scores, v)
    decay = jnp.exp(cum_last[:, :, 0])

    def step(state, inp):
        qn, kn, vn, dn = inp
        o = jnp.einsum('bihd,bhdv->bihv', qn, state)
        state = dn[..., None] * state + jnp.einsum('bjhd,bjhv->bhdv', kn, vn)
        return state, o

    xs = (jnp.moveaxis(q_t, 1, 0), jnp.moveaxis(k_last, 1, 0), jnp.moveaxis(v, 1, 0), jnp.moveaxis(decay, 1, 0))
    s0 = jnp.zeros((bsz, B_HEADS, B_DK, B_DV), f32)
    _, o_inter = lax.scan(step, s0, xs)
    o = o_intra + jnp.moveaxis(o_inter, 0, 1)
    return o.reshape(bsz, seq, B_HEADS, B_DV)


def _token_mixer(x, w_in, b_in, a_ws, a_bs, a_ln_g, a_ln_b, gla_w_lr, gla_b_lr, gla_gn_g, w_br_a, w_br_b, w_o):
    f32 = jnp.float32
    bsz, seq, _ = x.shape
    split_points = tuple(int(s) for s in np.cumsum(SPLITS)[:-1])
    z = jnp.einsum('btd,de->bte', x, w_in) + b_in
    a_u, a_v, q, k, v, r, lr, g_a, g_b = jnp.split(z, split_points, axis=-1)
    y_a = _spatial_gating(jax.nn.gelu(a_u, approximate=False), jax.nn.gelu(a_v, approximate=False),
                          a_ln_g, a_ln_b, a_ws, a_bs)
    o = _gla(q, k, v, lr, gla_w_lr, gla_b_lr)
    o = o * lax.rsqrt(jnp.mean(jnp.square(o), axis=-1, keepdims=True) + LN_EPS) * gla_gn_g.astype(f32)
    y_b = (o * jax.nn.silu(r.astype(f32)).reshape(bsz, seq, B_HEADS, B_DV)).reshape(bsz, seq, B_VAL_WIDTH).astype(x.dtype)
    merged = jax.nn.sigmoid(g_a) * (y_a @ w_br_a) + jax.nn.sigmoid(g_b) * (y_b @ w_br_b)
    return merged @ w_o


def _moe(x, w_router, b_router, w_gu, b_gu, w_down, b_down):
    f32 = jnp.float32
    bsz, seq, d = x.shape
    n_tok = bsz * seq
    xt = x.reshape(n_tok, d)
    n_assign = n_tok * TOP_K
    n_blocks = -(-(n_assign + N_EXPERTS * (MOE_BLOCK - 1)) // MOE_BLOCK)
    n_pad = n_blocks * MOE_BLOCK
    logits = (xt @ w_router + b_router).astype(f32)
    top_logits, top_idx = lax.top_k(logits, TOP_K)
    top_w = jax.nn.softmax(top_logits, axis=-1)
    flat_e = top_idx.reshape(-1)
    flat_tok = jnp.arange(n_assign, dtype=jnp.int32) // TOP_K
    flat_w = top_w.reshape(-1)
    order = jnp.argsort(flat_e)
    sorted_e = flat_e[order]
    counts = jnp.bincount(flat_e, length=N_EXPERTS)
    group_start = jnp.cumsum(counts) - counts
    padded = (counts + MOE_BLOCK - 1) // MOE_BLOCK * MOE_BLOCK
    padded_end = jnp.cumsum(padded)
    padded_start = padded_end - padded
    rank = jnp.arange(n_assign, dtype=jnp.int32) - group_start[sorted_e]
    dest = padded_start[sorted_e] + rank
    tok_buf = jnp.zeros((n_pad,), jnp.int32).at[dest].set(flat_tok[order])
    w_buf = jnp.zeros((n_pad,), f32).at[dest].set(flat_w[order])
    block_e = jnp.minimum(jnp.searchsorted(padded_end, jnp.arange(n_blocks) * MOE_BLOCK, side='right'), N_EXPERTS - 1)

    def block(y, inp):
        tok, w, e = inp
        xb = xt[tok]
        gu = xb @ w_gu[e] + b_gu[e]
        gate, lin = jnp.split(gu, 2, axis=-1)
        gate = jnp.minimum(gate, SWIGLU_LIMIT)
        lin = jnp.clip(lin, -SWIGLU_LIMIT, SWIGLU_LIMIT)
        h = gate * jax.nn.sigmoid(SWIGLU_ALPHA * gate) * (lin + 1)
        out = (h @ w_down[e] + b_down[e]).astype(f32)
        return y.at[tok].add(out * w[:, None]), None

    y0 = jnp.zeros((n_tok, d), f32)
    y, _ = lax.scan(block, y0, (tok_buf.reshape(n_blocks, MOE_BLOCK), w_buf.reshape(n_blocks, MOE_BLOCK), block_e))
    return y.reshape(bsz, seq, d).astype(x.dtype)


def setup_inputs(seed: int = 0) -> dict:
    key = jax.random.key(seed)
    ks = jax.random.split(key, 23)

    def nrm(k, shape, scale):
        return jax.random.normal(k, shape, jnp.float32) * scale

    L = DEPTH
    return {
        'x': nrm(ks[0], (BATCH, SEQ, D_MODEL), 1.0),
        'w_in': nrm(ks[1], (L, D_MODEL, D_IN), D_MODEL ** -0.5),
        'b_in': nrm(ks[2], (L, D_IN), 0.02),
        'a_ws': nrm(ks[3], (L, A_GROUPS, A_CHUNK, A_CHUNK), A_CHUNK ** -0.5),
        'a_bs': 1.0 + nrm(ks[4], (L, A_GROUPS, A_CHUNK), 0.01),
        'a_ln_g': 1.0 + nrm(ks[5], (L, A_GROUPS, A_GROUP_DIM), 0.01),
        'a_ln_b': nrm(ks[6], (L, A_GROUPS, A_GROUP_DIM), 0.01),
        'gla_w_lr': nrm(ks[7], (L, B_GATE_RANK, B_KEY_WIDTH), B_GATE_RANK ** -0.5),
        'gla_b_lr': nrm(ks[8], (L, B_KEY_WIDTH), 0.02),
        'gla_gn_g': 1.0 + nrm(ks[9], (L, B_HEADS, B_DV), 0.01),
        'w_br_a': nrm(ks[10], (L, A_WIDTH, D_MODEL), DEEPNORM_BETA * A_WIDTH ** -0.5),
        'w_br_b': nrm(ks[11], (L, B_VAL_WIDTH, D_MODEL), DEEPNORM_BETA * B_VAL_WIDTH ** -0.5),
        'w_o': nrm(ks[12], (L, D_MODEL, D_MODEL), DEEPNORM_BETA * D_MODEL ** -0.5),
        'ln1_g': 1.0 + nrm(ks[13], (L, D_MODEL), 0.01),
        'ln1_b': nrm(ks[14], (L, D_MODEL), 0.01),
        'w_router': nrm(ks[15], (L, D_MODEL, N_EXPERTS), D_MODEL ** -0.5),
        'b_router': nrm(ks[16], (L, N_EXPERTS), 0.01),
        'w_gu': nrm(ks[17], (L, N_EXPERTS, D_MODEL, 2 * D_EXPERT), D_MODEL ** -0.5),
        'b_gu': nrm(ks[18], (L, N_EXPERTS, 2 * D_EXPERT), 0.01),
        'w_down': nrm(ks[19], (L, N_EXPERTS, D_EXPERT, D_MODEL), DEEPNORM_BETA * D_EXPERT ** -0.5),
        'b_down': nrm(ks[20], (L, N_EXPERTS, D_MODEL), 0.01),
        'ln2_g': 1.0 + nrm(ks[21], (L, D_MODEL), 0.01),
        'ln2_b': nrm(ks[22], (L, D_MODEL), 0.01),
    }


def reference(x, w_in, b_in, a_ws, a_bs, a_ln_g, a_ln_b, gla_w_lr, gla_b_lr, gla_gn_g, w_br_a, w_br_b, w_o,
              ln1_g, ln1_b, w_router, b_router, w_gu, b_gu, w_down, b_down, ln2_g, ln2_b):
    for l in range(DEPTH):
        mix = _token_mixer(x, w_in[l], b_in[l], a_ws[l], a_bs[l], a_ln_g[l], a_ln_b[l], gla_w_lr[l], gla_b_lr[l],
                           gla_gn_g[l], w_br_a[l], w_br_b[l], w_o[l])
        x = _layer_norm(DEEPNORM_ALPHA * x + mix, ln1_g[l], ln1_b[l])
        ffn = _moe(x, w_router[l], b_router[l], w_gu[l], b_gu[l], w_down[l], b_down[l])
        x = _layer_norm(DEEPNORM_ALPHA * x + ffn, ln2_g[l], ln2_b[l])
    return x
```

```python
import os
import numpy as np
from contextlib import ExitStack
import concourse.bass as bass
import concourse.mybir as mybir
from concourse.bass_utils import run_bass_kernel_spmd

F32 = mybir.dt.float32
BF = mybir.dt.bfloat16
I32 = mybir.dt.int32
U8 = mybir.dt.uint8
AF = mybir.ActivationFunctionType
ALU = mybir.AluOpType

NCORES = 8
T = 2048
D = 4096
KD = 32
TT = 256
NS = TT // 128
NTT = T // TT
NGS = T // 128
DIN = 18448
C_AU, C_AV, C_Q, C_K, C_V, C_R, C_LR, C_GA, C_GB = 0, 2048, 4096, 5120, 6144, 8192, 10240, 10256, 14352
NE = 32
DE = 1536
CAP = 384
NSG = CAP // 128
ALPHA = 2.0 ** 0.25
EPS = 1e-5
CELL = 1024
NWB = 3
WBC = 33 * 256
YROWS = 4 * T + 128

STAGE = int(os.environ.get("KSTAGE", "9"))
NCR = int(os.environ.get("K_NCORES", "8"))
NTILES_DBG = int(os.environ.get("K_NTILES", str(T // 256)))
NEXP_DBG = int(os.environ.get("K_NEXP", "32"))
KCUT = int(os.environ.get("K_CUT", "99"))


class StopEmit(Exception):
    pass


class Buf:
    __slots__ = ("ap", "cells")

    def __init__(self, ap, cells):
        self.ap = ap
        self.cells = cells


class Trk:
    ENG = ("pe", "act", "dve", "pool", "sp")

    def __init__(self):
        self.streams = {e: [] for e in self.ENG}
        self.cnt = {e: 0 for e in self.ENG}
        self.waited = {}
        self.cell = {}
        self.dcount = []
        self.dstep = []

    def new_dsem(self, step=16):
        self.dcount.append(0)
        self.dstep.append(step)
        return len(self.dcount) - 1

    def op(self, eng, fn, reads=(), writes=(), dsem=None):
        deps = {}

        def add(k, v):
            if deps.get(k, 0) < v:
                deps[k] = v

        for b in reads:
            for c in b.cells:
                st = self.cell.get(c)
                if st is not None and st[0] is not None:
                    add(*st[0])
        for b in writes:
            for c in b.cells:
                st = self.cell.get(c)
                if st is not None:
                    if st[0] is not None:
                        add(*st[0])
                    for k, v in st[1].items():
                        add(k, v)
        waits = []
        for k, v in deps.items():
            if k == ("e", "pe") and eng == "pe":
                continue
            if self.waited.get((eng, k), 0) >= v:
                continue
            self.waited[(eng, k)] = v
            waits.append((k, v))
        if dsem is None:
            self.cnt[eng] += 1
            me = (("e", eng), self.cnt[eng])
        else:
            self.dcount[dsem] += self.dstep[dsem]
            me = (("d", dsem), self.dcount[dsem])
        self.streams[eng].append((waits, fn, dsem))
        for b in writes:
            for c in b.cells:
                self.cell[c] = [me, {}]
        for b in reads:
            for c in b.cells:
                st = self.cell.get(c)
                if st is None:
                    self.cell[c] = [None, {me[0]: me[1]}]
                else:
                    if st[1].get(me[0], 0) < me[1]:
                        st[1][me[0]] = me[1]
        return me

    def wait_all(self, eng, bufs):
        deps = {}
        for b in bufs:
            for c in b.cells:
                st = self.cell.get(c)
                if st is not None and st[0] is not None:
                    k, v = st[0]
                    if deps.get(k, 0) < v:
                        deps[k] = v
        waits = [(k, v) for k, v in deps.items()]
        self.streams[eng].append((waits, None, None))


def _cf_layout():
    lay = {}
    off = 0
    for name, n in [("identf", 128), ("ltincl", 128), ("utstrict", 128), ("mask01", 256), ("tokid", 16),
                    ("dump", 1), ("negdump", 1), ("iotac", CAP), ("onesf", 128), ("ltstrict", 128),
                    ("maska", 128), ("awst", 512), ("lnag", 16), ("lnab", 16), ("gng", 16),
                    ("ln1g", 32), ("ln1b", 32), ("wr", 1024), ("abs", 512), ("brow", 32), ("ci", 2)]:
        lay[name] = (off, n)
        off += n
    return lay, off


CFL, NCF = _cf_layout()


def _build_cf(inp):
    cf = np.zeros((128, NCF), np.float32)

    def put(name, arr):
        o, n = CFL[name]
        cf[: arr.shape[0], o:o + n] = arr

    p = np.arange(128)
    put("identf", np.eye(128, dtype=np.float32))
    same = (p[:, None] // 64) == (p[None, :] // 64)
    put("ltincl", ((p[:, None] <= p[None, :]) & same).astype(np.float32))
    put("utstrict", ((p[:, None] > p[None, :]) & same).astype(np.float32))
    jl = p % 64
    m = (jl[:, None] <= np.arange(64)[None, :]).astype(np.float32)
    put("mask01", np.tile(m, (1, 4)))
    put("tokid", (np.arange(16)[None, :] * 128 + p[:, None]).astype(np.float32))
    put("dump", np.full((128, 1), 1.0e6, np.float32))
    put("negdump", np.full((128, 1), -1.0e6, np.float32))
    put("iotac", np.tile(np.arange(CAP, dtype=np.float32)[None, :], (128, 1)))
    put("onesf", np.ones((128, 128), np.float32))
    put("ltstrict", (p[:, None] < p[None, :]).astype(np.float32))
    put("maska", (p[:, None] <= p[None, :]).astype(np.float32))
    aws = inp["a_ws"][0]
    put("awst", np.concatenate([aws[g].T for g in range(4)], axis=1))
    put("lnag", inp["a_ln_g"][0].reshape(16, 128).T)
    put("lnab", inp["a_ln_b"][0].reshape(16, 128).T)
    put("gng", inp["gla_gn_g"][0].reshape(16, 128).T)
    put("ln1g", inp["ln1_g"][0].reshape(32, 128).T)
    put("ln1b", inp["ln1_b"][0].reshape(32, 128).T)
    wr = inp["w_router"][0].reshape(32, 128, 32).transpose(1, 0, 2).reshape(128, 1024)
    put("wr", wr)
    put("abs", inp["a_bs"][0].reshape(1, 512))
    put("brow", inp["b_router"][0].reshape(1, 32))
    put("ci", (p[:, None] // 64 == np.arange(2)[None, :]).astype(np.float32))
    return cf


def _build_coef(c):
    co = np.zeros((128, 72), np.float32)
    for a in range(8):
        for b in range(8):
            co[:, a * 8 + b] = 1.0 if (a < b < c) else 0.0
        co[:, 64 + a] = 1.0 if a < c else 0.0
    return co


def build_program():
    nc = bass.Bass("TRN2", target_bir_lowering=False)
    trk = Trk()
    es = ExitStack()

    def dram_in(name, shape, dt=F32):
        return nc.dram_tensor(name, list(shape), dt, kind="ExternalInput")

    x_d = dram_in("x", [T, D])
    b_in_d = dram_in("b_in", [1, DIN])
    b_gu_d = dram_in("b_gu", [NE, 2 * DE])
    WSPEC = [("w_in_a", D, C_R), ("w_in_b", D, DIN - C_R), ("w_br_a", 2048, D), ("w_br_b", 2048, D), ("w_o", D, D)]
    if STAGE >= 6:
        WSPEC += [(f"w_gu_{j}_{h}", 8 * D, DE) for j in range(4) for h in range(2)]
        WSPEC += [(f"w_dn_{j}", 8 * DE, D) for j in range(4)]
    wfull = {}
    wshard = {}
    wbounce = {}
    for name, rows, cols in WSPEC:
        if NCR == 1:
            wfull[name] = dram_in(name, [rows, cols])
        else:
            wshard[name] = dram_in(name, [rows // NCR, cols])
            wbounce[name] = nc.dram_tensor(name + "_bn", [rows // NCR, cols], BF)
            wfull[name] = nc.dram_tensor(name + "_full", [rows, cols], BF)
    w_bra_d, w_brb_d, w_o_d = wfull["w_br_a"], wfull["w_br_b"], wfull["w_o"]
    b_dn_d = dram_in("b_down", [NE, D])
    ln1_d = dram_in("ln1gb", [2, D])
    ln2_d = dram_in("ln2gb", [2, D])
    cf_d = dram_in("cf32", [128, NCF])
    wlr_d = dram_in("wlr", [17, 1024])
    coef_d = dram_in("coef", [128, 72])
    out_d = nc.dram_tensor("out", [T, D], F32, kind="ExternalOutput")

    dbg = STAGE < 9
    xn32_d = nc.dram_tensor("xn32", [T, D], F32, kind="ExternalOutput") if dbg else nc.dram_tensor("xn32", [T, D], F32)
    xn16_d = nc.dram_tensor("xn16", [T, D], BF)
    ybuf_d = [nc.dram_tensor(f"ybuf{i}", [YROWS, 512], F32) for i in range(8)]
    rt_d = nc.dram_tensor("rtinfo", [128, NGS * 192], F32, kind="ExternalOutput") if dbg else nc.dram_tensor("rtinfo", [128, NGS * 192], F32)
    slin_d = nc.dram_tensor("slin", [128, 4104], F32)
    slall_d = nc.dram_tensor("slall", [NCORES * 128, 4104], F32)

    def dcell(name, n=1):
        return [("dr", name, i) for i in range(n)]

    XN32 = [Buf(None, dcell(f"xn32_{g}")) for g in range(NGS)]
    XN16 = [Buf(None, dcell(f"xn16_{g}")) for g in range(NGS)]
    YBUF = Buf(None, dcell("ybuf"))
    RTD = [Buf(None, dcell(f"rt_{g}")) for g in range(NGS)]
    SLIN = Buf(None, dcell("slin"))
    SLALL = Buf(None, dcell("slall"))

    ARENA_BYTES = 206 * 1024
    ar = es.enter_context(nc.sbuf_tensor("arena", [128, ARENA_BYTES], U8))
    ps = es.enter_context(nc.psum_tensor("ps", [128, 4096], F32))
    ps_bf = ps[:, :].bitcast(BF)

    def sb(off, nbytes, dt):
        sz = 4 if dt in (F32, I32) else 2
        ap = ar[:, off:off + nbytes].bitcast(dt)
        cells = [("sb", i) for i in range(off // CELL, (off + nbytes - 1) // CELL + 1)]
        return Buf(ap, cells)

    def psb(c0, n):
        cells = [("ps", i) for i in range(c0 // 128, (c0 + n - 1) // 128 + 1)]
        return Buf(ps[:, c0:c0 + n], cells)

    class Ar:
        def __init__(self, base):
            self.off = base

        def alloc(self, nbytes):
            o = self.off
            self.off += (nbytes + CELL - 1) // CELL * CELL
            return o

    A = Ar(0)
    o_cf = A.alloc(NCF * 4)
    CFB = sb(o_cf, NCF * 4, F32)

    def cf(name, rows=128, c0=0, n=None):
        o, nn = CFL[name]
        if n is None:
            n = nn - c0
        return CFB.ap[0:rows, o + c0:o + c0 + n]

    IDB = sb(A.alloc(256), 256, BF)
    ONESB = sb(A.alloc(1024), 1024, BF)
    LTSB = sb(A.alloc(256), 256, BF)
    WSTM = sb(A.alloc(1024), 1024, BF)
    T2 = sb(A.alloc(8192), 8192, F32)
    WLR = sb(A.alloc(2048), 2048, BF)
    COEF = sb(A.alloc(72 * 4), 72 * 4, F32)
    LOGD = sb(A.alloc(64), 64, F32)
    CARRY = sb(A.alloc(256), 256, F32)
    o_s32 = A.alloc(16384)
    S32 = [sb(o_s32 + i * 2048, 2048, F32) for i in range(8)]
    o_sbf = A.alloc(8192)
    SBF = [sb(o_sbf + i * 1024, 1024, BF) for i in range(8)]
    o_wb = [A.alloc(WBC * 2) for _ in range(NWB)]
    WB = [sb(o, WBC * 2, BF) for o in o_wb]
    WB_DS = [trk.new_dsem() for _ in range(NWB)]
    P0 = A.off

    T2v = T2.ap.rearrange("p (a b) -> p a b", a=16)
    WSTMv = WSTM.ap.rearrange("p (a b) -> p a b", a=4)

    def cut(n):
        if KCUT == n:
            raise StopEmit()

    wb_state = {"i": 0}

    def next_wb():
        i = wb_state["i"] % NWB
        wb_state["i"] += 1
        return i

    def load_w(i, src_ap, kc, ncols, bias_ap=None, dep="w_in"):
        v = WB[i].ap[:, 0:(kc + 1) * ncols].rearrange("p (a b) -> p a b", b=ncols)
        deps = [WD["w_in_a"], WD["w_in_b"]] if dep == "w_in" else [WD[dep]]
        trk.op("pool", lambda e: e.dma_start(out=v[:, 0:kc, :], in_=src_ap), reads=deps, writes=[WB[i]], dsem=WB_DS[i])
        if bias_ap is not None:
            trk.op("pool", lambda e: e.dma_start(out=v[0:1, kc, :], in_=bias_ap), writes=[WB[i]], dsem=WB_DS[i])
        return v

    def w_in_src(c0, n):
        if c0 < C_R:
            assert c0 + n <= C_R
            return wfull["w_in_a"].ap()[:, c0:c0 + n].rearrange("(kc p) n -> p kc n", p=128)
        return wfull["w_in_b"].ap()[:, c0 - C_R:c0 - C_R + n].rearrange("(kc p) n -> p kc n", p=128)

    def mm_group(out_ap, pairs):
        n = len(pairs)

        def fn(e):
            ins = None
            for i, (l, r) in enumerate(pairs):
                ins = e.matmul(out_ap, l, r, start=(i == 0), stop=(i == n - 1))
            return ins
        return fn

    def act(out, in_, func, bias=None, scale=None):
        kw = {}
        if bias is not None:
            kw["bias"] = bias
        if scale is not None:
            kw["scale"] = scale
        return lambda e: e.activation(out=out, in_=in_, func=func, **kw)

    def tt(out, in0, in1, op):
        return lambda e: e.tensor_tensor(out=out, in0=in0, in1=in1, op=op)

    def ts(out, in0, s1, op0, s2=None, op1=None):
        if op1 is None:
            return lambda e: e.tensor_scalar(out=out, in0=in0, scalar1=s1, scalar2=None, op0=op0)
        return lambda e: e.tensor_scalar(out=out, in0=in0, scalar1=s1, scalar2=s2, op0=op0, op1=op1)

    def stt(out, in0, scalar, in1, op0, op1):
        return lambda e: e.scalar_tensor_tensor(out=out, in0=in0, scalar=scalar, in1=in1, op0=op0, op1=op1)

    def cp(out, in_):
        return lambda e: e.tensor_copy(out=out, in_=in_)

    WD = {name: Buf(None, dcell("wf_" + name)) for name, _, _ in WSPEC}

    def gather_weight(name):
        rows, cols = wshard[name].shape[0], wshard[name].shape[1]
        ds = trk.new_dsem()
        BN = Buf(None, dcell("wb_" + name))
        rpc = max(1, (8 * 1024 * 1024) // (min(cols, 2048) * 4))
        for c0 in range(0, cols, 2048):
            c1 = min(cols, c0 + 2048)
            for r0 in range(0, rows, rpc):
                r1 = min(rows, r0 + rpc)
                trk.op("pool", lambda e, r0=r0, r1=r1, c0=c0, c1=c1: e.dma_start(
                    out=wbounce[name].ap()[r0:r1, c0:c1], in_=wshard[name].ap()[r0:r1, c0:c1]),
                    writes=[BN], dsem=ds)
        cc = trk.new_dsem(step=1)
        trk.op("pool", lambda e: e.collective_compute("AllGather", ALU.bypass, replica_groups=[list(range(NCORES))],
                                                      ins=[wbounce[name].ap().opt()], outs=[wfull[name].ap().opt()]),
               reads=[BN], writes=[WD[name]], dsem=cc)

    if NCR > 1:
        for nm, _, _ in WSPEC:
            gather_weight(nm)
        trk.wait_all("pool", [WD[nm] for nm, _, _ in WSPEC])

    DS_C = trk.new_dsem()
    DS_C2 = trk.new_dsem()
    trk.op("sp", lambda e: e.dma_start(out=CFB.ap, in_=cf_d.ap()), writes=[CFB], dsem=DS_C)
    DS_C3 = trk.new_dsem()
    trk.op("sp", lambda e: e.dma_start(out=COEF.ap, in_=coef_d.ap()), writes=[COEF], dsem=DS_C3)
    trk.op("pool", lambda e: e.dma_start(out=WLR.ap[0:17, :], in_=wlr_d.ap()), writes=[WLR], dsem=DS_C2)
    trk.op("dve", cp(IDB.ap, cf("identf")), reads=[CFB], writes=[IDB])
    trk.op("dve", cp(LTSB.ap, cf("ltstrict")), reads=[CFB], writes=[LTSB])
    trk.op("dve", lambda e: e.memset(ONESB.ap, 1.0), writes=[ONESB])
    awst = cf("awst").rearrange("p (a b) -> p a b", a=4)
    for g in range(4):
        trk.op("dve", tt(WSTMv[:, g, :], awst[:, g, :], cf("maska"), ALU.mult), reads=[CFB], writes=[WSTM])
    PS_R = psb(0, 512)
    PS_BS = psb(512, 512)
    for g in range(4):
        trk.op("pe", mm_group(PS_R.ap[:, g * 128:(g + 1) * 128], [(ONESB.ap[:, 0:128], WSTMv[:, g, :])]),
               reads=[ONESB, WSTM], writes=[PS_R])
    trk.op("pe", mm_group(PS_BS.ap, [(cf("onesf", rows=1), cf("abs", rows=1))]), reads=[CFB], writes=[PS_BS])
    BSS = sb(P0, 2048, F32)
    trk.op("dve", cp(BSS.ap, PS_BS.ap), reads=[PS_BS], writes=[BSS])
    for g in range(4):
        for j in range(4):
            blk = g * 4 + j
            trk.op("dve", stt(T2v[:, blk, :], PS_R.ap[:, g * 128:(g + 1) * 128], cf("lnab", c0=blk, n=1),
                              BSS.ap[:, g * 128:(g + 1) * 128], ALU.mult, ALU.add),
                   reads=[PS_R, BSS, CFB], writes=[T2])
    for i in range(8):
        trk.op("dve", lambda e, i=i: e.memset(S32[i].ap, 0.0), writes=[S32[i]])
    trk.op("dve", lambda e: e.memset(LOGD.ap, 1.0), writes=[LOGD])
    trk.op("dve", lambda e: e.memset(CARRY.ap, 0.0), writes=[CARRY])

    B = Ar(P0)
    o_xt = B.alloc(KD * TT * 2)
    o_scrA = B.alloc(8192)
    o_uy = B.alloc(16 * TT * 2)
    o_qt = B.alloc(8 * TT * 2)
    o_kt = B.alloc(8 * TT * 2)
    o_ktm = B.alloc(NS * 1024 * 2)
    o_kl = B.alloc(NS * 1024 * 2)
    o_vtm = B.alloc(NS * 2048 * 2)
    o_scrB = B.alloc(16384)
    o_sr = B.alloc(16 * TT * 2)
    o_lr = B.alloc(32 * TT * 2 // 1 if False else TT * 2)
    o_misc = B.alloc(8192)
    o_xp = B.alloc(4096)
    PH_END = B.off
    assert PH_END <= ARENA_BYTES, PH_END

    XT = sb(o_xt, KD * TT * 2, BF)
    XTv = XT.ap.rearrange("p (a b) -> p a b", a=KD)
    XTM = sb(o_scrA, 8192, BF)
    SP = sb(o_scrA, 4096, F32)
    EQ = sb(o_scrA + 4096, 4096, F32)
    EQv = EQ.ap.rearrange("p (a b) -> p a b", a=8)
    UY = sb(o_uy, 16 * TT * 2, BF)
    UYv = UY.ap.rearrange("p (a b) -> p a b", a=16)
    QT = sb(o_qt, 8 * TT * 2, BF)
    QTv = QT.ap.rearrange("p (a b) -> p a b", a=8)
    KT = sb(o_kt, 8 * TT * 2, BF)
    KTv = KT.ap.rearrange("p (a b) -> p a b", a=8)
    KTM = [sb(o_ktm + s * 2048, 2048, BF) for s in range(NS)]
    KL = [sb(o_kl + s * 2048, 2048, BF) for s in range(NS)]
    VTM = [sb(o_vtm + s * 4096, 4096, BF) for s in range(NS)]
    VH = [sb(o_scrB + s * 4096, 4096, BF) for s in range(NS)]
    VG = [sb(o_scrB + 8192 + i * 2048, 2048, F32) for i in range(2)]
    EK = sb(o_scrB, 4096, F32)
    EKv = EK.ap.rearrange("p (a b) -> p a b", a=8)
    EREV = sb(o_scrB + 4096, 4096, F32)
    SCT = sb(o_scrB + 8192, 512, BF)
    SQ = sb(o_scrB + 9216, 2048, BF)
    RR = sb(o_scrB + 11264, 1024, F32)
    RSTD = sb(o_scrB + 12288, 1024, F32)
    TMP = [sb(o_scrB + 13312 + i * 1024, 1024, F32) for i in range(2)]
    SR = sb(o_sr, 16 * TT * 2, BF)
    SRv = SR.ap.rearrange("p (a b) -> p a b", a=16)
    LR = sb(o_lr, TT * 2, BF)
    MISC = o_misc
    LNST = sb(MISC, 1024, F32)
    TMPA = sb(MISC + 1024, 2048, F32)
    TMPAv = TMPA.ap.rearrange("p (a b) -> p a b", a=4)
    SIG = [sb(MISC + 3072 + i * 1024, 1024, F32) for i in range(4)]
    XP = [sb(o_xp + i * 1024, 1024, F32) for i in range(4)]
    XP_DS = [trk.new_dsem() for _ in range(4)]
    MG = sb(o_qt, KD * TT * 2, BF)
    MGv = MG.ap.rearrange("p (a b) -> p a b", a=KD)
    H = [sb(o_xt + s * 16384, 16384, F32) for s in range(NS)]
    X1T = sb(o_vtm, 16384, F32)
    X1Tv = X1T.ap.rearrange("p (a b) -> p a b", a=KD)
    RT = sb(o_scrB + 8192, 4096, F32)
    XTM_DS = trk.new_dsem()
    XN32_DS = [trk.new_dsem() for _ in range(NS)]
    XN16_DS = [trk.new_dsem() for _ in range(NS)]
    RT_DS = trk.new_dsem()

    trk.op("dve", lambda e: e.memset(LR.ap[0:32, :], 1.0), writes=[LR])

    def bank(b, c0=0, n=512):
        return psb(b * 512 + c0, n)

    def load_xt(tb):
        for s in range(NS):
            r0 = tb + s * 128
            for hh in range(2):
                trk.op("pool", lambda e, r0=r0, hh=hh: e.dma_start(out=XTM.ap[:, hh * 2048:(hh + 1) * 2048],
                                                                 in_=x_d.ap()[r0:r0 + 128, hh * 2048:(hh + 1) * 2048]),
                       writes=[XTM], dsem=XTM_DS)
            for q in range(4):
                PB = bank(q)
                pbv = ps_bf[:, q * 1024:(q + 1) * 1024]

                def fn(e, q=q, pbv=pbv):
                    ins = None
                    for j in range(8):
                        kc = q * 8 + j
                        ins = e.transpose(out=pbv[:, j * 128:(j + 1) * 128], in_=XTM.ap[:, kc * 128:(kc + 1) * 128],
                                          identity=IDB.ap)
                    return ins
                trk.op("pe", fn, reads=[XTM, IDB], writes=[PB])
                dst = XTv[:, q * 8:(q + 1) * 8, s * 128:(s + 1) * 128]
                src = pbv.rearrange("p (a b) -> p a b", a=8)
                eng = "act" if q % 2 == 0 else "dve"
                if eng == "act":
                    trk.op("act", act(dst, src, AF.Identity), reads=[PB], writes=[XT])
                else:
                    trk.op("dve", cp(dst, src), reads=[PB], writes=[XT])

    fm_rot = {"i": 0}

    def inproj_fm(c0, ncols, evac, banks=(4, 5)):
        done = 0
        while done < ncols:
            n = min(256, ncols - done)
            i = next_wb()
            v = load_w(i, w_in_src(c0 + done, n), KD, n, b_in_d.ap()[0:1, c0 + done:c0 + done + n])
            for j0 in range(0, n, 128):
                m = min(128, n - j0)
                bk = banks[fm_rot["i"] % len(banks)]
                fm_rot["i"] += 1
                PB = bank(bk, 0, TT)
                pairs = [(v[:, kc, j0:j0 + m], XTv[:, kc, :]) for kc in range(KD)]
                pairs.append((v[0:1, KD, j0:j0 + m], ONESB.ap[0:1, 0:TT]))
                trk.op("pe", mm_group(PB.ap[0:m, :], pairs), reads=[WB[i], XT, ONESB], writes=[PB])
                evac((done + j0) // 128, PB, m)
            done += n

    tm_rot = {"i": 0}

    def inproj_tm(c0, ncols, evac, banks=(6, 7)):
        done = 0
        while done < ncols:
            n = min(256, ncols - done)
            i = next_wb()
            v = load_w(i, w_in_src(c0 + done, n), KD, n, b_in_d.ap()[0:1, c0 + done:c0 + done + n])
            for s in range(NS):
                bk = banks[tm_rot["i"] % len(banks)]
                tm_rot["i"] += 1
                PB = bank(bk, 0, n)
                pairs = [(XTv[:, kc, s * 128:(s + 1) * 128], v[:, kc, :]) for kc in range(KD)]
                pairs.append((ONESB.ap[0:1, 0:128], v[0:1, KD, :]))
                trk.op("pe", mm_group(PB.ap, pairs), reads=[WB[i], XT, ONESB], writes=[PB])
                evac(s, done, n, PB)
            done += n

    def k_v_lr_proj(with_fm_k):
        def ev_ktm(s, co, n, PB):
            trk.op("dve", cp(KTM[s].ap[:, co:co + n], PB.ap), reads=[PB], writes=[KTM[s]])

        def ev_kfm(blk, PB, m):
            trk.op("act", act(KTv[:, blk, :], PB.ap, AF.Identity), reads=[PB], writes=[KT])
        for done in range(0, 1024, 256):
            i = next_wb()
            v = load_w(i, w_in_src(C_K + done, 256), KD, 256, b_in_d.ap()[0:1, C_K + done:C_K + done + 256])
            for s in range(NS):
                bk = (6, 7)[tm_rot["i"] % 2]
                tm_rot["i"] += 1
                PB = bank(bk, 0, 256)
                pairs = [(XTv[:, kc, s * 128:(s + 1) * 128], v[:, kc, :]) for kc in range(KD)]
                pairs.append((ONESB.ap[0:1, 0:128], v[0:1, KD, :]))
                trk.op("pe", mm_group(PB.ap, pairs), reads=[WB[i], XT, ONESB], writes=[PB])
                ev_ktm(s, done, 256, PB)
            if with_fm_k:
                for j0 in (0, 128):
                    bk = (4, 5)[fm_rot["i"] % 2]
                    fm_rot["i"] += 1
                    PB = bank(bk, 0, TT)
                    pairs = [(v[:, kc, j0:j0 + 128], XTv[:, kc, :]) for kc in range(KD)]
                    pairs.append((v[0:1, KD, j0:j0 + 128], ONESB.ap[0:1, 0:TT]))
                    trk.op("pe", mm_group(PB.ap, pairs), reads=[WB[i], XT, ONESB], writes=[PB])
                    ev_kfm((done + j0) // 128, PB, 128)

        def ev_v(s, co, n, PB):
            trk.op("act", act(VTM[s].ap[:, co:co + n], PB.ap, AF.Identity), reads=[PB], writes=[VTM[s]])
        inproj_tm(C_V, 2048, ev_v)

        def ev_lr(blk, PB, m):
            trk.op("act", act(LR.ap[0:16, :], PB.ap[0:16, :], AF.Identity), reads=[PB], writes=[LR])
        inproj_fm(C_LR, 16, ev_lr)

    def gates(s, full):
        PG = [bank(0), bank(1)]
        for hf in range(2):
            trk.op("pe", mm_group(PG[hf].ap, [(LR.ap[0:17, s * 128:(s + 1) * 128], WLR.ap[0:17, hf * 512:(hf + 1) * 512])]),
                   reads=[LR, WLR], writes=[PG[hf]])
            trk.op("act", act(SP.ap[:, hf * 512:(hf + 1) * 512], PG[hf].ap, AF.Exp, scale=-1.0), reads=[PG[hf]], writes=[SP])
        cut(40)
        trk.op("act", act(SP.ap, SP.ap, AF.Ln, bias=1.0, scale=1.0), reads=[SP], writes=[SP])
        cut(41)
        PRV = [bank(2), bank(3)]
        for hf in range(2):
            trk.op("pe", mm_group(PRV[hf].ap, [(cf("utstrict"), SP.ap[:, hf * 512:(hf + 1) * 512])]),
                   reads=[CFB, SP], writes=[PRV[hf]])
            trk.op("act", act(EREV.ap[:, hf * 512:(hf + 1) * 512], PRV[hf].ap, AF.Exp, scale=-1.0 / 16), reads=[PRV[hf]], writes=[EREV])
        cut(42)
        trk.op("dve", tt(KL[s].ap, KTM[s].ap, EREV.ap, ALU.mult), reads=[KTM[s], EREV], writes=[KL[s]])
        cut(43)
        PC = [bank(0), bank(1)]
        for hf in range(2):
            def fn(e, hf=hf):
                ins = None
                for b4 in range(4):
                    blk = hf * 4 + b4
                    ins = e.matmul(PC[hf].ap[:, b4 * 128:(b4 + 1) * 128], SP.ap[:, blk * 128:(blk + 1) * 128], cf("ltincl"),
                                   start=True, stop=True)
                return ins
            trk.op("pe", fn, reads=[SP, CFB], writes=[PC[hf]])
        cut(44)
        for hf in range(2):
            trk.op("act", act(EQ.ap[:, hf * 512:(hf + 1) * 512], PC[hf].ap, AF.Exp, scale=-1.0 / 16), reads=[PC[hf]], writes=[EQ])
            if full:
                trk.op("act", act(EK.ap[:, hf * 512:(hf + 1) * 512], PC[hf].ap, AF.Exp, scale=1.0 / 16), reads=[PC[hf]], writes=[EK])
        for col in (63, 127):
            trk.op("dve", tt(LOGD.ap[:, 0:8], LOGD.ap[:, 0:8], EQv[:, :, col], ALU.mult), reads=[LOGD, EQ], writes=[LOGD])
        cut(45)
        if full:
            trk.op("dve", tt(QTv[:, :, s * 128:(s + 1) * 128], QTv[:, :, s * 128:(s + 1) * 128], EQv, ALU.mult),
                   reads=[QT, EQ], writes=[QT])
            trk.op("dve", tt(KTv[:, :, s * 128:(s + 1) * 128], KTv[:, :, s * 128:(s + 1) * 128], EKv, ALU.mult),
                   reads=[KT, EK], writes=[KT])

    kv_rot = {"i": 0}

    def state_update(s, cc, with_bf):
        p0 = cc * 64
        for h in range(4):
            for e2 in range(2):
                blk = 2 * h + e2
                bk = (6, 7)[kv_rot["i"] % 2]
                kv_rot["i"] += 1
                PB = bank(bk)
                trk.op("pe", mm_group(PB.ap, [(KL[s].ap[p0:p0 + 64, blk * 128:(blk + 1) * 128],
                                               VTM[s].ap[p0:p0 + 64, h * 512:(h + 1) * 512])]),
                       reads=[KL[s], VTM[s]], writes=[PB])
                dec = EQv[:, blk, cc * 64 + 63:cc * 64 + 64]
                trk.op("dve", stt(S32[blk].ap, S32[blk].ap, dec, PB.ap, ALU.mult, ALU.add),
                       reads=[S32[blk], EQ, PB], writes=[S32[blk]])
                if with_bf:
                    trk.op("act", act(SBF[blk].ap, S32[blk].ap, AF.Identity), reads=[S32[blk]], writes=[SBF[blk]])

    def gla_chunk(s, cc):
        p0 = cc * 64
        t0 = s * 128 + cc * 64
        PSC = bank(2, 0, 256)

        def fsc(e):
            ins = None
            for h in range(4):
                for e2 in range(2):
                    blk = 2 * h + e2
                    ins = e.matmul(PSC.ap[p0:p0 + 64, h * 64:(h + 1) * 64], KTv[:, blk, t0:t0 + 64], QTv[:, blk, t0:t0 + 64],
                                   start=(e2 == 0), stop=(e2 == 1))
            return ins
        trk.op("pe", fsc, reads=[KT, QT], writes=[PSC])
        trk.op("dve", tt(SCT.ap[p0:p0 + 64, :], PSC.ap[p0:p0 + 64, :], cf("mask01")[p0:p0 + 64, :], ALU.mult),
               reads=[PSC, CFB], writes=[SCT])
        PO = psb(0, 1024)
        pov = PO.ap.rearrange("p (a b c) -> p a b c", a=4, b=4)

        def fo(e):
            ins = None
            for vb in range(4):
                for h in range(4):
                    o_ap = pov[:, vb, h, :]
                    e.matmul(o_ap, VTM[s].ap[p0:p0 + 64, h * 512 + vb * 128:h * 512 + (vb + 1) * 128],
                             SCT.ap[p0:p0 + 64, h * 64:(h + 1) * 64], start=True, stop=False)
                    for e2 in range(2):
                        blk = 2 * h + e2
                        ins = e.matmul(o_ap, SBF[blk].ap[:, vb * 128:(vb + 1) * 128], QTv[:, blk, t0:t0 + 64],
                                       start=False, stop=(e2 == 1))
            return ins
        trk.op("pe", fo, reads=[VTM[s], SCT, QT] + SBF, writes=[PO])
        trk.op("act", act(SQ.ap, PO.ap, AF.Square), reads=[PO], writes=[SQ])
        PSS = bank(3, 0, 256)
        trk.op("pe", mm_group(PSS.ap, [(ONESB.ap[:, 0:128], SQ.ap[:, vb * 256:(vb + 1) * 256]) for vb in range(4)]),
               reads=[ONESB, SQ], writes=[PSS])
        trk.op("act", act(RR.ap, PSS.ap, AF.Sqrt, bias=EPS, scale=1.0 / 512), reads=[PSS], writes=[RR])
        trk.op("dve", lambda e: e.reciprocal(out=RSTD.ap, in_=RR.ap), reads=[RR], writes=[RSTD])
        for vb in range(4):
            tmp = TMP[vb % 2]
            trk.op("dve", tt(tmp.ap, PO.ap[:, vb * 256:(vb + 1) * 256], RSTD.ap, ALU.mult), reads=[PO, RSTD], writes=[tmp])
            yv = SRv[:, vb::4, t0:t0 + 64]
            trk.op("dve", tt(yv, tmp.ap.rearrange("p (a b) -> p a b", a=4), yv, ALU.mult), reads=[tmp, SR], writes=[SR])
        state_update(s, cc, True)

    YZ = Buf(None, dcell("ybuf_zero"))
    if STAGE >= 6:
        ZT = sb(o_xt, 16384, F32)
        DS_YZ = trk.new_dsem()
        trk.op("dve", lambda e: e.memset(ZT.ap, 0.0), writes=[ZT])
        ztv = ZT.ap.rearrange("p (g n) -> p g n", g=8)
        for db in range(8):
            for g0 in range(0, YROWS // 128, 8):
                g1 = min(YROWS // 128, g0 + 8)
                trk.op("sp", lambda e, db=db, g0=g0, g1=g1: e.dma_start(
                    out=ybuf_d[db].ap()[g0 * 128:g1 * 128, :].rearrange("(g p) n -> p g n", p=128), in_=ztv[:, 0:g1 - g0, :]),
                    reads=[ZT], writes=[YZ], dsem=DS_YZ)

    if NCR > 1:
        for ti in range(NTT):
            load_xt(ti * TT)
            k_v_lr_proj(False)
            for s in range(NS):
                gates(s, False)
                for cc in range(2):
                    state_update(s, cc, False)
        DS_SL = trk.new_dsem()
        S32ALL = sb(o_s32, 16384, F32)
        trk.op("sp", lambda e: e.dma_start(out=slin_d.ap()[:, 0:4096], in_=S32ALL.ap), reads=[S32ALL], writes=[SLIN], dsem=DS_SL)
        trk.op("sp", lambda e: e.dma_start(out=slin_d.ap()[:, 4096:4104], in_=LOGD.ap[:, 0:8]), reads=[LOGD], writes=[SLIN], dsem=DS_SL)
        DS_CC = trk.new_dsem(step=1)
        trk.op("pool", lambda e: e.collective_compute("AllGather", ALU.bypass, replica_groups=[list(range(NCORES))],
                                                      ins=[slin_d.ap().opt()], outs=[slall_d.ap().opt()]),
               reads=[SLIN], writes=[SLALL], dsem=DS_CC)
        LDA = sb(P0, 256, F32)
        EE = sb(P0 + 1024, 256, F32)
        MM_ = sb(P0 + 2048, 256, F32)
        SLOC = [sb(P0 + 4096 + i * 16384, 16384, F32) for i in range(2)]
        DS_LD = trk.new_dsem()
        DS_SLOC = [trk.new_dsem() for _ in range(2)]
        LDAv = LDA.ap.rearrange("p (a b) -> p a b", a=8)
        EEv = EE.ap.rearrange("p (a b) -> p a b", a=8)
        MMv = MM_.ap.rearrange("p (a b) -> p a b", a=8)
        for c in range(8):
            trk.op("sp", lambda e, c=c: e.dma_start(out=LDAv[:, c, :], in_=slall_d.ap()[c * 128:(c + 1) * 128, 4096:4104]),
                   reads=[SLALL], writes=[LDA], dsem=DS_LD)
        for a in range(8):
            trk.op("dve", ts(MMv[:, a, :], LDAv[:, 0, :], 0.0, ALU.mult, COEF.ap[:, 64 + a:65 + a], ALU.add), reads=[LDA, COEF], writes=[MM_])
            for b in range(8):
                trk.op("dve", ts(EEv[:, a, :], LDAv[:, b, :], -1.0, ALU.add, COEF.ap[:, a * 8 + b:a * 8 + b + 1], ALU.mult),
                       reads=[LDA, COEF], writes=[EE])
                trk.op("dve", stt(MMv[:, a, :], EEv[:, a, :], 1.0, MMv[:, a, :], ALU.add, ALU.mult), reads=[EE, MM_], writes=[MM_])
        for i in range(8):
            trk.op("dve", lambda e, i=i: e.memset(S32[i].ap, 0.0), writes=[S32[i]])
        for c in range(8):
            sl = SLOC[c % 2]
            trk.op("sp", lambda e, c=c, sl=sl: e.dma_start(out=sl.ap, in_=slall_d.ap()[c * 128:(c + 1) * 128, 0:4096]),
                   reads=[SLALL], writes=[sl], dsem=DS_SLOC[c % 2])
            for blk in range(8):
                trk.op("dve", stt(S32[blk].ap, sl.ap[:, blk * 512:(blk + 1) * 512], MMv[:, c, blk:blk + 1], S32[blk].ap, ALU.mult, ALU.add),
                       reads=[sl, MM_, S32[blk]], writes=[S32[blk]])
    for blk in range(8):
        trk.op("act", act(SBF[blk].ap, S32[blk].ap, AF.Identity), reads=[S32[blk]], writes=[SBF[blk]])

    def phase_b_tile(ti):
        tb = ti * TT
        cut(0)
        load_xt(tb)
        cut(1)

        def ev_u(blk, PB, m):
            trk.op("act", act(UYv[:, blk, :], PB.ap, AF.Gelu), reads=[PB], writes=[UY])
        inproj_fm(C_AU, 2048, ev_u)

        def ev_av(s, co, n, PB):
            g = co // 512
            half = (co % 512) // 256
            vg = VG[s]
            trk.op("act", act(vg.ap[:, half * 256:(half + 1) * 256], PB.ap, AF.Gelu), reads=[PB], writes=[vg])
            if half == 1:
                st = LNST.ap[:, s * 16:s * 16 + 6]
                mv = LNST.ap[:, s * 16 + 8:s * 16 + 10]
                rs = LNST.ap[:, s * 16 + 10:s * 16 + 11]
                trk.op("dve", lambda e: e.bn_stats(out=st, in_=vg.ap), reads=[vg], writes=[LNST])
                trk.op("dve", lambda e: e.bn_aggr(out=mv, in_=st), reads=[LNST], writes=[LNST])
                trk.op("act", act(rs, mv[:, 1:2], AF.Sqrt, bias=EPS, scale=1.0), reads=[LNST], writes=[LNST])
                trk.op("dve", lambda e: e.reciprocal(out=rs, in_=rs), reads=[LNST], writes=[LNST])
                trk.op("dve", ts(VH[s].ap[:, g * 512:(g + 1) * 512], vg.ap, mv[:, 0:1], ALU.subtract, rs, ALU.mult),
                       reads=[vg, LNST], writes=[VH[s]])
        inproj_tm(C_AV, 2048, ev_av)
        for s in range(NS):
            for g in range(4):
                PB = bank(g % 4)

                def fsp(e, s=s, g=g, PB=PB):
                    ins = None
                    for j in range(4):
                        ins = e.matmul(PB.ap[:, j * 128:(j + 1) * 128], VH[s].ap[:, g * 512 + j * 128:g * 512 + (j + 1) * 128],
                                       WSTMv[:, g, :], start=True, stop=True)
                    return ins
                trk.op("pe", fsp, reads=[VH[s], WSTM], writes=[PB])
                for j in range(4):
                    blk = g * 4 + j
                    trk.op("dve", stt(TMPAv[:, j, :], PB.ap[:, j * 128:(j + 1) * 128], cf("lnag", c0=blk, n=1), T2v[:, blk, :],
                                      ALU.mult, ALU.add), reads=[PB, CFB, T2], writes=[TMPA])
                    uyv = UYv[:, blk, s * 128:(s + 1) * 128]
                    trk.op("dve", tt(uyv, TMPAv[:, j, :], uyv, ALU.mult), reads=[TMPA, UY], writes=[UY])

        cut(2)

        def ev_q(blk, PB, m):
            trk.op("act", act(QTv[:, blk, :], PB.ap, AF.Identity, scale=1.0 / 16), reads=[PB], writes=[QT])
        inproj_fm(C_Q, 1024, ev_q)
        k_v_lr_proj(True)

        def ev_r(blk, PB, m):
            tmp = SIG[blk % 2]
            trk.op("act", act(tmp.ap, PB.ap, AF.Silu), reads=[PB], writes=[tmp])
            trk.op("dve", ts(SRv[:, blk, :], tmp.ap, cf("gng", c0=blk, n=1), ALU.mult), reads=[tmp, CFB], writes=[SR])
        inproj_fm(C_R, 2048, ev_r)
        cut(3)
        for s in range(NS):
            gates(s, True)
            cut(4)
            for cc in range(2):
                gla_chunk(s, cc)
                cut(5)
        cut(6)
        if STAGE == 3 and ti == 0:
            return

        for dmp in range(16):
            ia = next_wb()
            va = load_w(ia, w_in_src(C_GA + dmp * 256, 256), KD, 256, b_in_d.ap()[0:1, C_GA + dmp * 256:C_GA + (dmp + 1) * 256])
            ib = next_wb()
            vb_ = load_w(ib, w_in_src(C_GB + dmp * 256, 256), KD, 256, b_in_d.ap()[0:1, C_GB + dmp * 256:C_GB + (dmp + 1) * 256])
            ic = next_wb()
            vc = WB[ic].ap[:, 0:32 * 256].rearrange("p (a b) -> p a b", b=256)
            trk.op("pool", lambda e, vc=vc, dmp=dmp: e.dma_start(
                out=vc[:, 0:16, :], in_=w_bra_d.ap()[:, dmp * 256:(dmp + 1) * 256].rearrange("(kc p) n -> p kc n", p=128)),
                reads=[WD["w_br_a"]], writes=[WB[ic]], dsem=WB_DS[ic])
            trk.op("pool", lambda e, vc=vc, dmp=dmp: e.dma_start(
                out=vc[:, 16:32, :], in_=w_brb_d.ap()[:, dmp * 256:(dmp + 1) * 256].rearrange("(kc p) n -> p kc n", p=128)),
                reads=[WD["w_br_b"]], writes=[WB[ic]], dsem=WB_DS[ic])
            for j in range(2):
                dm = dmp * 2 + j
                j0 = j * 128
                PGA, PGB, PPA, PPB = bank(4 * (j % 2) + 0, 0, TT), bank(4 * (j % 2) + 1, 0, TT), bank(4 * (j % 2) + 2, 0, TT), bank(4 * (j % 2) + 3, 0, TT)
                pa = [(va[:, kc, j0:j0 + 128], XTv[:, kc, :]) for kc in range(KD)] + [(va[0:1, KD, j0:j0 + 128], ONESB.ap[0:1, 0:TT])]
                trk.op("pe", mm_group(PGA.ap, pa), reads=[WB[ia], XT, ONESB], writes=[PGA])
                pb_ = [(vb_[:, kc, j0:j0 + 128], XTv[:, kc, :]) for kc in range(KD)] + [(vb_[0:1, KD, j0:j0 + 128], ONESB.ap[0:1, 0:TT])]
                trk.op("pe", mm_group(PGB.ap, pb_), reads=[WB[ib], XT, ONESB], writes=[PGB])
                trk.op("pe", mm_group(PPA.ap, [(vc[:, kc, j0:j0 + 128], UYv[:, kc, :]) for kc in range(16)]), reads=[WB[ic], UY], writes=[PPA])
                trk.op("pe", mm_group(PPB.ap, [(vc[:, 16 + kc, j0:j0 + 128], SRv[:, kc, :]) for kc in range(16)]), reads=[WB[ic], SR], writes=[PPB])
                sa, sb_, t1, t2 = SIG
                trk.op("act", act(sa.ap, PGA.ap, AF.Sigmoid), reads=[PGA], writes=[sa])
                trk.op("act", act(sb_.ap, PGB.ap, AF.Sigmoid), reads=[PGB], writes=[sb_])
                trk.op("dve", tt(t1.ap, sa.ap, PPA.ap, ALU.mult), reads=[sa, PPA], writes=[t1])
                trk.op("dve", tt(t2.ap, sb_.ap, PPB.ap, ALU.mult), reads=[sb_, PPB], writes=[t2])
                trk.op("dve", tt(MGv[:, dm, :], t1.ap, t2.ap, ALU.add), reads=[t1, t2], writes=[MG])

        cut(7)
        xp_rot = 0
        for cg in range(16):
            i = next_wb()
            v = load_w(i, w_o_d.ap()[:, cg * 256:(cg + 1) * 256].rearrange("(kc p) n -> p kc n", p=128), KD, 256, dep="w_o")
            for s in range(NS):
                xp = xp_rot % 4
                xp_rot += 1
                r0 = tb + s * 128
                trk.op("sp", lambda e, xp=xp, r0=r0, cg=cg: e.dma_start(out=XP[xp].ap, in_=x_d.ap()[r0:r0 + 128, cg * 256:(cg + 1) * 256]),
                       writes=[XP[xp]], dsem=XP_DS[xp])
                PB = bank((0, 1, 2, 3)[(cg * NS + s) % 4], 0, 256)
                trk.op("pe", mm_group(PB.ap, [(MGv[:, kc, s * 128:(s + 1) * 128], v[:, kc, :]) for kc in range(KD)]),
                       reads=[MG, WB[i]], writes=[PB])
                trk.op("dve", stt(H[s].ap[:, cg * 256:(cg + 1) * 256], XP[xp].ap, ALPHA, PB.ap, ALU.mult, ALU.add),
                       reads=[XP[xp], PB], writes=[H[s]])
        cut(8)
        for s in range(NS):
            gs = ti * NS + s
            st = LNST.ap[:, 0:48].rearrange("p (a b) -> p a b", a=8)
            for c8 in range(8):
                trk.op("dve", lambda e, c8=c8, s=s: e.bn_stats(out=st[:, c8, :], in_=H[s].ap[:, c8 * 512:(c8 + 1) * 512]),
                       reads=[H[s]], writes=[LNST])
            mv = LNST.ap[:, 48:50]
            rs = LNST.ap[:, 50:51]
            nmr = LNST.ap[:, 51:52]
            trk.op("dve", lambda e: e.bn_aggr(out=mv, in_=LNST.ap[:, 0:48]), reads=[LNST], writes=[LNST])
            trk.op("act", act(rs, mv[:, 1:2], AF.Sqrt, bias=EPS, scale=1.0), reads=[LNST], writes=[LNST])
            trk.op("dve", lambda e: e.reciprocal(out=rs, in_=rs), reads=[LNST], writes=[LNST])
            trk.op("dve", ts(nmr, mv[:, 0:1], rs, ALU.mult, -1.0, ALU.mult), reads=[LNST], writes=[LNST])
            trk.op("act", act(H[s].ap, H[s].ap, AF.Identity, bias=nmr, scale=rs), reads=[H[s], LNST], writes=[H[s]])
            r0 = tb + s * 128
            trk.op("sp", lambda e, s=s, r0=r0: e.dma_start(out=xn32_d.ap()[r0:r0 + 128, :], in_=H[s].ap),
                   reads=[H[s]], writes=[XN32[gs]], dsem=XN32_DS[s])
            for hh in range(2):
                trk.op("pool", lambda e, s=s, r0=r0, hh=hh: e.dma_start(out=xn16_d.ap()[r0:r0 + 128, hh * 2048:(hh + 1) * 2048],
                                                                      in_=H[s].ap[:, hh * 2048:(hh + 1) * 2048]),
                       reads=[H[s]], writes=[XN16[gs]], dsem=XN16_DS[s])
            if STAGE <= 4:
                continue
            for q in range(8):
                PB = bank(4 + q % 4)

                def ftr(e, q=q, s=s, PB=PB):
                    ins = None
                    for j in range(4):
                        kc = q * 4 + j
                        ins = e.transpose(out=PB.ap[:, j * 128:(j + 1) * 128], in_=H[s].ap[:, kc * 128:(kc + 1) * 128], identity=cf("identf"))
                    return ins
                trk.op("pe", ftr, reads=[H[s], CFB], writes=[PB])
                for j in range(4):
                    kc = q * 4 + j
                    trk.op("act", act(X1Tv[:, kc, :], PB.ap[:, j * 128:(j + 1) * 128], AF.Identity,
                                      bias=cf("ln1b", c0=kc, n=1), scale=cf("ln1g", c0=kc, n=1)), reads=[PB, CFB], writes=[X1T])
            PL = bank(0, 0, 32)
            wrv = cf("wr").rearrange("p (a b) -> p a b", a=32)
            pairs = [(X1Tv[:, kc, :], wrv[:, kc, :]) for kc in range(KD)] + [(cf("onesf", rows=1), cf("brow", rows=1))]
            trk.op("pe", mm_group(PL.ap, pairs), reads=[X1T, CFB], writes=[PL])
            rt = RT.ap
            LG, MX, EX, MS, WG, C0, C1, VL = (rt[:, 0:32], rt[:, 32:40], rt[:, 64:96], rt[:, 96:128], rt[:, 128:160],
                                              rt[:, 160:192], rt[:, 192:224], rt[:, 256:384])
            NMX, DEN = rt[:, 40:41], rt[:, 41:42]
            trk.op("dve", cp(LG, PL.ap), reads=[PL], writes=[RT])
            trk.op("dve", lambda e: e.max(out=MX, in_=LG), reads=[RT], writes=[RT])
            trk.op("dve", ts(MS, LG, MX[:, 3:4], ALU.is_ge), reads=[RT], writes=[RT])
            trk.op("dve", ts(NMX, MX[:, 0:1], -1.0, ALU.mult), reads=[RT], writes=[RT])
            trk.op("act", act(EX, LG, AF.Exp, bias=NMX, scale=1.0), reads=[RT], writes=[RT])
            trk.op("dve", tt(EX, EX, MS, ALU.mult), reads=[RT], writes=[RT])
            trk.op("dve", lambda e: e.reduce_sum(out=DEN, in_=EX, axis=mybir.AxisListType.X), reads=[RT], writes=[RT])
            trk.op("dve", lambda e: e.reciprocal(out=DEN, in_=DEN), reads=[RT], writes=[RT])
            trk.op("dve", ts(WG, EX, DEN, ALU.mult), reads=[RT], writes=[RT])
            trk.op("dve", cp(C0, MS), reads=[RT], writes=[RT])
            src, dst = C0, C1
            for sh in (1, 2, 4, 8, 16):
                trk.op("dve", cp(dst[:, 0:sh], src[:, 0:sh]), reads=[RT], writes=[RT])
                trk.op("dve", tt(dst[:, sh:32], src[:, sh:32], src[:, 0:32 - sh], ALU.add), reads=[RT], writes=[RT])
                src, dst = dst, src
            KP = dst
            trk.op("dve", tt(KP, src, MS, ALU.subtract), reads=[RT], writes=[RT])
            MB = rt[:, 224:240].bitcast(BF)
            trk.op("dve", cp(MB, MS), reads=[RT], writes=[RT])
            PRK = bank(1, 0, 64)
            trk.op("pe", mm_group(PRK.ap[:, 0:32], [(LTSB.ap, MB)]), reads=[LTSB, RT], writes=[PRK])
            trk.op("pe", mm_group(PRK.ap[:, 32:64], [(ONESB.ap[:, 0:128], MB)]), reads=[ONESB, RT], writes=[PRK])
            INFO = rt[:, 384:576]
            trk.op("dve", cp(INFO[:, 0:32], MS), reads=[RT], writes=[RT])
            trk.op("dve", tt(INFO[:, 32:64], PRK.ap[:, 0:32], CARRY.ap[:, 0:32], ALU.add), reads=[PRK, CARRY], writes=[RT])
            trk.op("dve", tt(CARRY.ap[:, 0:32], CARRY.ap[:, 0:32], PRK.ap[:, 32:64], ALU.add), reads=[PRK, CARRY], writes=[CARRY])
            vals = INFO[:, 64:192].rearrange("p (a b) -> p a b", b=4)
            tok = cf("tokid", c0=gs, n=1)
            trk.op("dve", ts(vals[:, :, 0], MS, 0.0, ALU.mult, tok, ALU.add), reads=[RT, CFB], writes=[RT])
            trk.op("dve", ts(vals[:, :, 1], KP, float(T), ALU.mult, tok, ALU.add), reads=[RT, CFB], writes=[RT])
            trk.op("dve", cp(vals[:, :, 2], WG), reads=[RT], writes=[RT])
            trk.op("dve", ts(vals[:, :, 3], MS, 0.0, ALU.mult, 1.0, ALU.add), reads=[RT], writes=[RT])
            trk.op("sp", lambda e, gs=gs, INFO=INFO: e.dma_start(out=rt_d.ap()[:, gs * 192:(gs + 1) * 192], in_=INFO),
                   reads=[RT], writes=[RTD[gs]], dsem=RT_DS)

    if STAGE >= 3:
        ntiles = min(NTT, NTILES_DBG) if STAGE >= 4 else 1
        try:
            for ti in range(ntiles):
                phase_b_tile(ti)
        except StopEmit:
            pass

    YB_CELLS = []
    if STAGE >= 6:
        M = Ar(P0)
        o_wb4 = M.alloc(WBC * 2)
        WBM = WB + [sb(o_wb4, WBC * 2, BF)]
        WBM_DS = WB_DS + [trk.new_dsem()]
        NWM = len(WBM)
        INFOA = sb(M.alloc(NGS * 192 * 4), NGS * 192 * 4, F32)
        infov = INFOA.ap.rearrange("p (g c) -> p g c", g=NGS)
        XG = [sb(M.alloc(8192), 8192, BF) for _ in range(2)]
        XG_DS = [trk.new_dsem() for _ in range(2)]
        assert o_sbf == o_s32 + 16384
        XGT = sb(o_s32, KD * CAP * 2, BF)
        XGTv = XGT.ap.rearrange("p (a b) -> p a b", a=KD)
        HT = sb(M.alloc(12 * CAP * 2), 12 * CAP * 2, BF)
        HTv = HT.ap.rearrange("p (a b) -> p a b", a=12)
        GU = [[sb(M.alloc(CAP * 4), CAP * 4, F32) for _ in range(4)] for _ in range(2)]
        OUTB = [sb(M.alloc(2048), 2048, F32) for _ in range(4)]
        OUT_DS = [trk.new_dsem() for _ in range(4)]
        SEL = [sb(M.alloc(1024), 1024, F32) for _ in range(4)]
        SLOT = [sb(M.alloc(1024), 1024, F32) for _ in range(2)]
        assert M.off <= ARENA_BYTES, M.off
        DS_INFO = trk.new_dsem()
        trk.op("sp", lambda e: e.dma_start(out=INFOA.ap, in_=rt_d.ap()), reads=RTD, writes=[INFOA], dsem=DS_INFO)

        wlist = []
        for ex in range(NE):
            for fp in range(6):
                wlist.append(("g", ex, fp))
                wlist.append(("l", ex, fp))
            for db in range(8):
                wlist.append(("d", ex, db))
        wstate = {"issued": 0}

        def issue_block(bi):
            kind, ex, idx = wlist[bi]
            i = bi % NWM
            if kind in ("g", "l"):
                c0 = idx * 256 + (DE if kind == "l" else 0)
                wn = f"w_gu_{ex % 4}_{1 if kind == 'l' else 0}"
                rr = (ex // 4) * D
                v = WBM[i].ap[:, 0:33 * 256].rearrange("p (a b) -> p a b", b=256)
                trk.op("pool", lambda e: e.dma_start(
                    out=v[:, 0:KD, :], in_=wfull[wn].ap()[rr:rr + D, idx * 256:(idx + 1) * 256].rearrange("(kc p) n -> p kc n", p=128)),
                    reads=[WD[wn]], writes=[WBM[i]], dsem=WBM_DS[i])
                trk.op("pool", lambda e: e.dma_start(out=v[0:1, KD, :], in_=b_gu_d.ap()[ex:ex + 1, c0:c0 + 256]),
                       writes=[WBM[i]], dsem=WBM_DS[i])
            else:
                v = WBM[i].ap[:, 0:13 * 512].rearrange("p (a b) -> p a b", b=512)
                wn = f"w_dn_{ex % 4}"
                rr = (ex // 4) * DE
                trk.op("pool", lambda e: e.dma_start(
                    out=v[:, 0:12, :], in_=wfull[wn].ap()[rr:rr + DE, idx * 512:(idx + 1) * 512].rearrange("(kc p) n -> p kc n", p=128)),
                    reads=[WD[wn]], writes=[WBM[i]], dsem=WBM_DS[i])
                trk.op("pool", lambda e: e.dma_start(out=v[0:1, 12, :], in_=b_dn_d.ap()[ex:ex + 1, idx * 512:(idx + 1) * 512]),
                       writes=[WBM[i]], dsem=WBM_DS[i])

        def use_block(bi, base=None):
            if base is None:
                base = bi
            lim = min(len(wlist), base + NWM)
            while wstate["issued"] < lim:
                issue_block(wstate["issued"])
                wstate["issued"] += 1
            i = bi % NWM
            kind = wlist[bi][0]
            if kind in ("g", "l"):
                return i, WBM[i].ap[:, 0:33 * 256].rearrange("p (a b) -> p a b", b=256)
            return i, WBM[i].ap[:, 0:13 * 512].rearrange("p (a b) -> p a b", b=512)

        bc_cache = {}

        def bc_reg(e):
            if "r" not in bc_cache:
                bc_cache["r"] = e.to_reg(YROWS - 1)
            return bc_cache["r"]

        out_rot = 0
        sel_rot = 0
        bi = 0
        if NEXP_DBG < NE:
            dbgm_d = nc.dram_tensor("dbgm", [128, 32 + 512 + 12 * CAP // 2 + KD * CAP // 2], F32, kind="ExternalOutput")
            DBG_DS2 = trk.new_dsem()
            DBGB = Buf(None, dcell("dbgm"))
        for ex in range(min(NE, NEXP_DBG)):
            sl = SLOT[ex % 2]
            slf = sl.ap[:, 0:12].rearrange("p (a b) -> p a b", a=3)
            gidx = sl.ap[:, 16:19].bitcast(I32)
            didx = sl.ap[:, 20:23].bitcast(I32)
            dtmp = sl.ap[:, 24:27]
            wj = sl.ap[:, 28:31]
            PI = bank(3, 496, 12)
            for sg in range(NSG):
                for gs in range(NGS):
                    se = SEL[sel_rot % 4]
                    sel_rot += 1
                    trk.op("dve", ts(se.ap[:, 0:128], cf("iotac", c0=sg * 128, n=128), infov[:, gs, 32 + ex:33 + ex], ALU.is_equal,
                                     infov[:, gs, ex:ex + 1], ALU.mult), reads=[CFB, INFOA], writes=[se])
                    trk.op("pe", (lambda se=se, sg=sg, gs=gs, ex=ex: (lambda e: e.matmul(
                        PI.ap[:, sg * 4:(sg + 1) * 4], se.ap[:, 0:128],
                        infov[:, gs, 64 + ex * 4:68 + ex * 4], start=(gs == 0), stop=(gs == NGS - 1))))(),
                        reads=[se, INFOA], writes=[PI])
            trk.op("act", act(sl.ap[:, 0:12], PI.ap, AF.Identity), reads=[PI], writes=[sl])
            trk.op("dve", cp(gidx, slf[:, :, 0]), reads=[sl], writes=[sl])
            trk.op("dve", ts(dtmp, slf[:, :, 3], cf("negdump"), ALU.mult, cf("dump"), ALU.add), reads=[sl, CFB], writes=[sl])
            trk.op("dve", tt(dtmp, dtmp, slf[:, :, 1], ALU.add), reads=[sl], writes=[sl])
            trk.op("dve", cp(didx, dtmp), reads=[sl], writes=[sl])
            trk.op("dve", cp(wj, slf[:, :, 2]), reads=[sl], writes=[sl])
            for sg in range(NSG):
                xg = XG[sg % 2]
                trk.op("pool", lambda e, xg=xg, sg=sg, gidx=gidx: e.indirect_dma_start(
                    out=xg.ap, out_offset=None, in_=xn16_d.ap(),
                    in_offset=bass.IndirectOffsetOnAxis(ap=gidx[:, sg:sg + 1], axis=0)),
                    reads=[sl] + XN16, writes=[xg], dsem=XG_DS[sg % 2])
                for q in range(4):
                    PB = bank(4 + q % 2)
                    pbv = ps_bf[:, (4 + q % 2) * 1024:(5 + q % 2) * 1024]

                    def ftr(e, q=q, xg=xg, pbv=pbv):
                        ins = None
                        for j in range(8):
                            kc = q * 8 + j
                            ins = e.transpose(out=pbv[:, j * 128:(j + 1) * 128], in_=xg.ap[:, kc * 128:(kc + 1) * 128], identity=IDB.ap)
                        return ins
                    trk.op("pe", ftr, reads=[xg, IDB], writes=[PB])
                    for j in range(8):
                        kc = q * 8 + j
                        trk.op("act", act(XGTv[:, kc, sg * 128:(sg + 1) * 128], pbv[:, j * 128:(j + 1) * 128], AF.Identity,
                                          bias=cf("ln1b", c0=kc, n=1), scale=cf("ln1g", c0=kc, n=1)), reads=[PB, CFB], writes=[XGT])
            if NEXP_DBG < NE and ex == 0:
                trk.op("sp", lambda e, sl=sl: e.dma_start(out=dbgm_d.ap()[:, 0:32], in_=sl.ap[:, 0:32]), reads=[sl], writes=[DBGB], dsem=DBG_DS2)
                trk.op("sp", lambda e: e.dma_start(out=dbgm_d.ap()[:, 544 + 12 * CAP // 2:544 + 12 * CAP // 2 + KD * CAP // 2].bitcast(BF), in_=XGT.ap), reads=[XGT], writes=[DBGB], dsem=DBG_DS2)
            for fp in range(6):
                ig, vg_ = use_block(bi)
                il, vl_ = use_block(bi + 1, base=bi)
                bi += 2
                for j in range(2):
                    fb = fp * 2 + j
                    j0 = j * 128
                    PGt, PLn = bank(0 + 2 * (fb % 2), 0, CAP), bank(1 + 2 * (fb % 2), 0, CAP)
                    pg = [(vg_[:, kc, j0:j0 + 128], XGTv[:, kc, :]) for kc in range(KD)] + [(vg_[0:1, KD, j0:j0 + 128], ONESB.ap[0:1, 0:CAP])]
                    trk.op("pe", mm_group(PGt.ap, pg), reads=[WBM[ig], XGT, ONESB], writes=[PGt])
                    pl = [(vl_[:, kc, j0:j0 + 128], XGTv[:, kc, :]) for kc in range(KD)] + [(vl_[0:1, KD, j0:j0 + 128], ONESB.ap[0:1, 0:CAP])]
                    trk.op("pe", mm_group(PLn.ap, pl), reads=[WBM[il], XGT, ONESB], writes=[PLn])
                    G, SGm, L, GS = GU[fb % 2]
                    trk.op("dve", ts(G.ap, PGt.ap, 7.0, ALU.min), reads=[PGt], writes=[G])
                    trk.op("act", act(SGm.ap, G.ap, AF.Sigmoid, scale=1.702), reads=[G], writes=[SGm])
                    trk.op("dve", ts(L.ap, PLn.ap, 7.0, ALU.min, -7.0, ALU.max), reads=[PLn], writes=[L])
                    trk.op("dve", tt(GS.ap, G.ap, SGm.ap, ALU.mult), reads=[G, SGm], writes=[GS])
                    trk.op("dve", stt(HTv[:, fb, :], L.ap, 1.0, GS.ap, ALU.add, ALU.mult), reads=[L, GS], writes=[HT])
            for db in range(8):
                iw, vd = use_block(bi)
                bi += 1
                for sg in range(NSG):
                    PB = bank(4 + (db * NSG + sg) % 4)
                    pr = [(HTv[:, kc, sg * 128:(sg + 1) * 128], vd[:, kc, :]) for kc in range(12)] + [(ONESB.ap[0:1, 0:128], vd[0:1, 12, :])]
                    trk.op("pe", mm_group(PB.ap, pr), reads=[HT, WBM[iw], ONESB], writes=[PB])
                    ob = out_rot % 4
                    out_rot += 1
                    trk.op("act", act(OUTB[ob].ap, PB.ap, AF.Identity, scale=wj[:, sg:sg + 1]), reads=[PB, sl], writes=[OUTB[ob]])
                    if NEXP_DBG < NE and ex == 0 and db == 0 and sg == 0:
                        trk.op("sp", lambda e, ob=ob: e.dma_start(out=dbgm_d.ap()[:, 32:544], in_=OUTB[ob].ap), reads=[OUTB[ob]], writes=[DBGB], dsem=DBG_DS2)
                        trk.op("sp", lambda e: e.dma_start(out=dbgm_d.ap()[:, 544:544 + 12 * CAP // 2].bitcast(BF), in_=HT.ap), reads=[HT], writes=[DBGB], dsem=DBG_DS2)
                    ycell = Buf(None, [("dr", "yb", len(YB_CELLS))])
                    YB_CELLS.append(ycell)
                    trk.op("pool", lambda e, ob=ob, db=db, sg=sg, didx=didx: e.indirect_dma_start(
                        out=ybuf_d[db].ap(), out_offset=bass.IndirectOffsetOnAxis(ap=didx[:, sg:sg + 1], axis=0),
                        in_=OUTB[ob].ap, in_offset=None, bounds_check=bc_reg(e), oob_is_err=False),
                        reads=[OUTB[ob], sl, YZ], writes=[ycell], dsem=OUT_DS[ob])

    if STAGE >= 7:
        Fz = Ar(o_s32)
        LNB = [sb(Fz.alloc(16384), 16384, F32) for _ in range(4)]
        ACC = [sb(Fz.alloc(16384), 16384, F32) for _ in range(2)]
        YK = [sb(Fz.alloc(16384), 16384, F32) for _ in range(2)]
        LNSTF = sb(Fz.alloc(1024), 1024, F32)
        assert Fz.off <= ARENA_BYTES
        trk.wait_all("sp", YB_CELLS)
        DS_LNB = [trk.new_dsem() for _ in range(4)]
        ACC_DS = [trk.new_dsem() for _ in range(2)]
        YK_DS = [trk.new_dsem() for _ in range(2)]
        OUTS_DS = [trk.new_dsem() for _ in range(2)]
        OUTD = [Buf(None, dcell(f"out_{g}")) for g in range(NGS)]
        for i, (src, row) in enumerate([(ln1_d, 0), (ln1_d, 1), (ln2_d, 0), (ln2_d, 1)]):
            trk.op("sp", lambda e, i=i, src=src, row=row: e.dma_start(out=LNB[i].ap, in_=src.ap()[row:row + 1, :].partition_broadcast(128)),
                   writes=[LNB[i]], dsem=DS_LNB[i])
        yk_rot = 0
        for gs in range(NGS):
            a = ACC[gs % 2]
            r0 = gs * 128
            trk.op("sp", lambda e, a=a, r0=r0: e.dma_start(out=a.ap, in_=xn32_d.ap()[r0:r0 + 128, :]),
                   reads=[XN32[gs]], writes=[a], dsem=ACC_DS[gs % 2])
            trk.op("dve", tt(a.ap, a.ap, LNB[0].ap, ALU.mult), reads=[a, LNB[0]], writes=[a])
            trk.op("pool", tt(a.ap, a.ap, LNB[1].ap, ALU.add), reads=[a, LNB[1]], writes=[a])
            for k in range(4):
                y = YK[yk_rot % 2]
                yds = YK_DS[yk_rot % 2]
                yk_rot += 1
                for db in range(8):
                    trk.op("sp", lambda e, y=y, k=k, db=db, r0=r0: e.dma_start(
                        out=y.ap[:, db * 512:(db + 1) * 512], in_=ybuf_d[db].ap()[k * T + r0:k * T + r0 + 128, :]),
                        writes=[y], dsem=yds)
                if k == 0:
                    trk.op("dve", stt(a.ap, a.ap, ALPHA, y.ap, ALU.mult, ALU.add), reads=[a, y], writes=[a])
                else:
                    trk.op("pool" if k % 2 else "dve", tt(a.ap, a.ap, y.ap, ALU.add), reads=[a, y], writes=[a])
            st = LNSTF.ap[:, 0:48].rearrange("p (a b) -> p a b", a=8)
            for c8 in range(8):
                trk.op("dve", lambda e, c8=c8, a=a: e.bn_stats(out=st[:, c8, :], in_=a.ap[:, c8 * 512:(c8 + 1) * 512]),
                       reads=[a], writes=[LNSTF])
            mv = LNSTF.ap[:, 48:50]
            rs = LNSTF.ap[:, 50:51]
            nmr = LNSTF.ap[:, 51:52]
            trk.op("dve", lambda e: e.bn_aggr(out=mv, in_=LNSTF.ap[:, 0:48]), reads=[LNSTF], writes=[LNSTF])
            trk.op("act", act(rs, mv[:, 1:2], AF.Sqrt, bias=EPS, scale=1.0), reads=[LNSTF], writes=[LNSTF])
            trk.op("dve", lambda e: e.reciprocal(out=rs, in_=rs), reads=[LNSTF], writes=[LNSTF])
            trk.op("dve", ts(nmr, mv[:, 0:1], rs, ALU.mult, -1.0, ALU.mult), reads=[LNSTF], writes=[LNSTF])
            trk.op("act", act(a.ap, a.ap, AF.Identity, bias=nmr, scale=rs), reads=[a, LNSTF], writes=[a])
            trk.op("dve", tt(a.ap, a.ap, LNB[2].ap, ALU.mult), reads=[a, LNB[2]], writes=[a])
            trk.op("pool", tt(a.ap, a.ap, LNB[3].ap, ALU.add), reads=[a, LNB[3]], writes=[a])
            trk.op("sp", lambda e, a=a, r0=r0: e.dma_start(out=out_d.ap()[r0:r0 + 128, :], in_=a.ap),
                   reads=[a], writes=[OUTD[gs]], dsem=OUTS_DS[gs % 2])
        trk.wait_all("sp", OUTD)
        if NEXP_DBG < NE:
            trk.wait_all("sp", [DBGB])
    else:
        trk.wait_all("sp", XN32 + RTD)
        if STAGE == 3:
            DBG_DS = trk.new_dsem()
            OD = Buf(None, dcell("outdbg"))
            trk.op("sp", lambda e: e.dma_start(out=out_d.ap()[0:128, 0:2048].bitcast(BF), in_=UY.ap), reads=[UY], writes=[OD], dsem=DBG_DS)
            trk.op("sp", lambda e: e.dma_start(out=out_d.ap()[128:256, 0:2048].bitcast(BF), in_=SR.ap), reads=[SR], writes=[OD], dsem=DBG_DS)
            trk.wait_all("sp", [OD])

    esem = {en: es.enter_context(nc.semaphore(f"sem_{en}")) for en in Trk.ENG}
    dsems = [es.enter_context(nc.semaphore(f"dsem{i}")) for i in range(len(trk.dcount))]
    block = es.enter_context(nc.Block())

    def semobj(k):
        return esem[k[1]] if k[0] == "e" else dsems[k[1]]

    def body(en):
        def f(e):
            for waits, fn, dsem in trk.streams[en]:
                for k, v in waits:
                    e.wait_ge(semobj(k), v)
                if fn is None:
                    continue
                ins = fn(e)
                if dsem is None:
                    ins.then_inc(esem[en], 1)
                elif trk.dstep[dsem] == 1:
                    ins.then_inc(dsems[dsem])
                else:
                    ins.then_inc(dsems[dsem], 16)
        return f

    block.tensor(body("pe"))
    block.scalar(body("act"))
    block.vector(body("dve"))
    block.gpsimd(body("pool"))
    block.sync(body("sp"))
    es.close()
    print("instr counts", {k: len(v) for k, v in trk.streams.items()}, "dsems", len(trk.dcount), flush=True)
    return nc


_NC_CACHE = {}


def kernel(**inp):
    x = inp["x"][0]
    cfm = _build_cf(inp)
    wlr = np.concatenate([inp["gla_w_lr"][0], inp["gla_b_lr"][0][None, :]], axis=0).astype(np.float32)
    f32 = lambda a: np.ascontiguousarray(a, dtype=np.float32)
    small = {
        "b_in": f32(inp["b_in"][0][None, :]), "b_gu": f32(inp["b_gu"][0]), "b_down": f32(inp["b_down"][0]),
        "ln1gb": f32(np.stack([inp["ln1_g"][0], inp["ln1_b"][0]])), "ln2gb": f32(np.stack([inp["ln2_g"][0], inp["ln2_b"][0]])),
        "cf32": f32(cfm), "wlr": f32(wlr),
    }
    wgu = inp["w_gu"][0]
    wdn = inp["w_down"][0]
    moe = STAGE >= 6

    def big_for(c):
        d = {}
        rs = (lambda n: slice(c * (n // NCR), (c + 1) * (n // NCR))) if NCR > 1 else (lambda n: slice(0, n))
        d["w_in_a"] = f32(inp["w_in"][0][rs(D), :C_R])
        d["w_in_b"] = f32(inp["w_in"][0][rs(D), C_R:])
        d["w_br_a"] = f32(inp["w_br_a"][0][rs(2048)])
        d["w_br_b"] = f32(inp["w_br_b"][0][rs(2048)])
        d["w_o"] = f32(inp["w_o"][0][rs(D)])
        for j in range(4 if moe else 0):
            for h in range(2):
                nm = f"w_gu_{j}_{h}"
                if NCR > 1:
                    d[nm] = f32(wgu[4 * c + j, :, h * DE:(h + 1) * DE])
                elif moe:
                    d[nm] = f32(wgu[j::4, :, h * DE:(h + 1) * DE]).reshape(8 * D, DE)
                else:
                    d[nm] = np.zeros((8 * D, DE), np.float32)
            nm = f"w_dn_{j}"
            if NCR > 1:
                d[nm] = f32(wdn[4 * c + j])
            elif moe:
                d[nm] = f32(wdn[j::4]).reshape(8 * DE, D)
            else:
                d[nm] = np.zeros((8 * DE, D), np.float32)
        return d

    if "nc" not in _NC_CACHE:
        _NC_CACHE["nc"] = build_program()
    nc = _NC_CACHE["nc"]
    in_maps = []
    for c in range(NCR):
        m = dict(small)
        m["x"] = f32(x[c * T:(c + 1) * T])
        m["coef"] = _build_coef(c)
        m.update(big_for(c))
        in_maps.append(m)
    res = run_bass_kernel_spmd(nc, in_maps, core_ids=list(range(NCR)))
    kernel.last = res
    out = np.concatenate([r["out"] for r in res.results], axis=0)[None]
    return out.astype(np.float32)
```
